# Optimizing a Trainium2 kernel written in Bass

```python
import jax
import jax.numpy as jnp
from jax import lax
import numpy as np

D_MODEL = 1024
BATCH = 2
SEQ = 8192
DEPTH = 1

GRID_W = 64
CTX_LEN = 256
A_HEAD_DIM = 64
A_HEADS = D_MODEL // A_HEAD_DIM
D_A = A_HEADS * A_HEAD_DIM
LORA_DECAY = 64
LORA_AAA = 64
LORA_GATE = 160
GN_EPS = 64e-5
B_BLOCK_DIM = 64
B_BLOCKS = D_MODEL // B_BLOCK_DIM
D_B = B_BLOCKS * B_BLOCK_DIM
CONV_W = 5
LRU_C = 8.0
N_GROUPS = 4
EXPERTS_PER_GROUP = 8
N_EXPERTS = N_GROUPS * EXPERTS_PER_GROUP
TOP_K = 2
D_EXPERT = 512
MOE_BLOCK = 128
A_SLAB = 3 * D_A + LORA_DECAY + LORA_AAA + LORA_GATE
B_SLAB = 2 * D_B
D_IN = A_SLAB + B_SLAB + 2 * D_MODEL
RWKV_SPLITS = (D_A, 2 * D_A, 3 * D_A, 3 * D_A + LORA_DECAY, 3 * D_A + LORA_DECAY + LORA_AAA)
LN_EPS = 1e-5
ALPHA = (2 * DEPTH) ** 0.25
BETA = (8 * DEPTH) ** -0.25

kernel_name = 'hybrid_rwkv7_rglru_hmoe_flow_block'


def layer_norm(x, g=None, b=None):
    xf = x.astype(jnp.float32)
    mu = jnp.mean(xf, -1, keepdims=True)
    var = jnp.mean(jnp.square(xf - mu), -1, keepdims=True)
    y = ((xf - mu) * lax.rsqrt(var + LN_EPS)).astype(x.dtype)
    if g is not None:
        y = y * g + b
    return y


def flip(t):
    return t[:, ::-1]


def grid_shift(z):
    b_, l_, ch = z.shape
    rows = l_ // GRID_W
    zg = z.reshape(b_, rows, GRID_W, ch // 4, 4)
    left = jnp.pad(zg[:, :, :-1, :, 0], ((0, 0), (0, 0), (1, 0), (0, 0)))
    right = jnp.pad(zg[:, :, 1:, :, 1], ((0, 0), (0, 0), (0, 1), (0, 0)))
    up = jnp.pad(zg[:, :-1, :, :, 2], ((0, 0), (1, 0), (0, 0), (0, 0)))
    down = jnp.pad(zg[:, 1:, :, :, 3], ((0, 0), (0, 1), (0, 0), (0, 0)))
    return jnp.stack([left, right, up, down], -1).reshape(b_, l_, ch)


def seq_shift(z):
    b_, l_, ch = z.shape
    zs = z.reshape(b_, l_, ch // 2, 2)
    prev = jnp.pad(zs[:, :-1, :, 0], ((0, 0), (1, 0), (0, 0)))
    nxt = jnp.pad(zs[:, 1:, :, 1], ((0, 0), (0, 1), (0, 0)))
    return jnp.stack([prev, nxt], -1).reshape(b_, l_, ch)


def depthwise_conv(x, w, b):
    y = lax.conv_general_dilated(x, w[:, None, :], window_strides=(1,),
                                 padding=[(CONV_W // 2, CONV_W // 2)],
                                 dimension_numbers=('NWC', 'WIO', 'NWC'),
                                 feature_group_count=x.shape[-1])
    return y + b


def wkv7_scan(r, w, k, v, kk, a, s0):
    xs = tuple(jnp.moveaxis(t, 1, 0) for t in (r, w, k, v, kk, a))

    def step(s, inp):
        r_t, w_t, k_t, v_t, kk_t, a_t = inp
        s_kk = jnp.einsum('bhvk,bhk->bhv', s, kk_t)
        s = (s * w_t[:, :, None, :] - s_kk[..., None] * (kk_t * a_t)[:, :, None, :]
             + v_t[..., None] * k_t[:, :, None, :])
        return s, jnp.einsum('bhvk,bhk->bhv', s, r_t)

    s_final, ys = lax.scan(step, s0, xs)
    return jnp.moveaxis(ys, 0, 1), s_final


def rwkv_mix(za, shift_fn, mu_a, w0, w2, a0, a2, g2, k_k, k_a, r_k, gn_g, gn_b, s0_f, s0_b):
    b_, l_, _ = za.shape
    za = za + mu_a * (shift_fn(za) - za)
    r, k, v, wd, ad, gd = jnp.split(za, RWKV_SPLITS, axis=-1)
    g = jax.nn.sigmoid(gd) @ g2
    w_log = -jax.nn.softplus(-(w0[:, None, None, :] + jnp.einsum('blr,erd->ebld', jnp.tanh(wd), w2))) - 0.5
    decay = jnp.exp(-jnp.exp(w_log))
    a = jax.nn.sigmoid(a0[:, None, None, :] + jnp.einsum('blr,erd->ebld', ad, a2))
    k_mod = k * (1.0 + (a - 1.0) * k_a)

    def heads(t):
        return t.reshape(t.shape[:-1] + (A_HEADS, A_HEAD_DIM))

    kk32 = heads(k * k_k).astype(jnp.float32)
    kk = (kk32 * lax.rsqrt(jnp.sum(jnp.square(kk32), -1, keepdims=True) + 1e-12)).astype(k.dtype)
    r_h, v_h = heads(r), heads(v)
    decay_h, a_h, k_h = heads(decay), heads(a), heads(k_mod)
    y_f, s_f = wkv7_scan(r_h, decay_h[0], k_h[0], v_h, kk, a_h[0], s0_f)
    y_b, s_b = wkv7_scan(flip(r_h), flip(decay_h[1]), flip(k_h[1]), flip(v_h), flip(kk), flip(a_h[1]), s0_b)
    y = y_f + flip(y_b)
    y32 = y.astype(jnp.float32)
    mu = jnp.mean(y32, -1, keepdims=True)
    var = jnp.mean(jnp.square(y32 - mu), -1, keepdims=True)
    yn = ((y32 - mu) * lax.rsqrt(var + GN_EPS)).astype(y.dtype).reshape(b_, l_, D_A) * gn_g + gn_b
    bonus = jnp.sum(r_h * (k_h[0] + k_h[1]) * r_k, -1, keepdims=True) * v_h
    return (yn + bonus.reshape(b_, l_, D_A)) * g, s_f, s_b


def linear_scan(a, u, h0):
    def combine(e1, e2):
        a1, u1 = e1
        a2_, u2 = e2
        return a1 * a2_, a2_ * u1 + u2

    a_cum, h = lax.associative_scan(combine, (a, u), axis=1)
    return h + a_cum * h0[:, None, :]


def rglru_mix(zb, conv_w, conv_b, wa, ba, wx, bx, lam, h0_f, h0_b):
    b_, l_, _ = zb.shape
    xb, gb = jnp.split(zb, 2, axis=-1)
    xb = depthwise_conv(xb, conv_w, conv_b)
    gate = jax.nn.gelu(gb)
    xh = xb.reshape(b_, l_, B_BLOCKS, B_BLOCK_DIM)
    rg = jax.nn.sigmoid(jnp.einsum('blhi,ehij->eblhj', xh, wa).reshape(2, b_, l_, D_B) + ba[:, None, None, :])
    ig = jax.nn.sigmoid(jnp.einsum('blhi,ehij->eblhj', xh, wx).reshape(2, b_, l_, D_B) + bx[:, None, None, :])
    log_a = -LRU_C * rg * jax.nn.softplus(-lam)[:, None, None, :]
    a = jnp.exp(log_a)
    u = jnp.sqrt(-jnp.expm1(2.0 * log_a)) * ig * xb
    h_f = linear_scan(a[0], u[0], h0_f)
    h_b = flip(linear_scan(flip(a[1]), flip(u[1]), h0_b))
    return (h_f + h_b) * gate, h_f[:, -1], h_b[:, 0]


def token_mix(h, shift_fn, w_in, p_a, p_b, w_o, rw, lru, states):
    slab = h @ w_in
    za = slab[..., :A_SLAB]
    zb = slab[..., A_SLAB:A_SLAB + B_SLAB]
    ga, gb = jnp.split(slab[..., A_SLAB + B_SLAB:], 2, axis=-1)
    s_f, s_b, h_f, h_b = states
    y_a, s_f, s_b = rwkv_mix(za, shift_fn, *rw, s_f, s_b)
    y_b, h_f, h_b = rglru_mix(zb, *lru, h_f, h_b)
    m = jax.nn.sigmoid(ga) * (y_a @ p_a) + jax.nn.sigmoid(gb) * (y_b @ p_b)
    return m @ w_o, (s_f, s_b, h_f, h_b)


def grouped_experts(xt, eid, wts, w1, w3, w2):
    t_, d_ = xt.shape
    n_assign = eid.shape[0]
    tok = jnp.arange(n_assign) // TOP_K
    order = jnp.argsort(eid)
    e_sorted = eid[order]
    counts = jnp.bincount(eid, length=N_EXPERTS)
    padded = (counts + MOE_BLOCK - 1) // MOE_BLOCK * MOE_BLOCK
    pad_end = jnp.cumsum(padded)
    pad_start = pad_end - padded
    start = jnp.cumsum(counts) - counts
    dest = pad_start[e_sorted] + jnp.arange(n_assign) - start[e_sorted]
    n_blocks = -(-n_assign // MOE_BLOCK) + N_EXPERTS
    n_pad = n_blocks * MOE_BLOCK
    buf_tok = jnp.full((n_pad,), t_, jnp.int32).at[dest].set(tok[order].astype(jnp.int32))
    buf_w = jnp.zeros((n_pad,), xt.dtype).at[dest].set(wts[order].astype(xt.dtype))
    blk_e = jnp.minimum(jnp.searchsorted(pad_end, jnp.arange(n_blocks) * MOE_BLOCK, side='right'), N_EXPERTS - 1)
    x_pad = jnp.concatenate([xt, jnp.zeros((1, d_), xt.dtype)], 0)
    xs = x_pad[buf_tok].reshape(n_blocks, MOE_BLOCK, d_)

    def expert_block(args):
        xb, e = args
        return (jax.nn.silu(xb @ w1[e]) * (xb @ w3[e])) @ w2[e]

    ys = lax.map(expert_block, (xs, blk_e)).reshape(n_pad, d_) * buf_w[:, None]
    return jnp.zeros((t_ + 1, d_), ys.dtype).at[buf_tok].add(ys)[:t_]


def hier_moe(h, rg_w, rg_b, re_w, re_b, w1, w3, w2):
    b_, l_, d_ = h.shape
    xt = h.reshape(b_ * l_, d_)
    glog = (xt @ rg_w + rg_b).astype(jnp.float32)
    gsel = jnp.argmax(glog, -1)
    p_g = jnp.take_along_axis(jax.nn.softmax(glog, -1), gsel[:, None], -1)
    elog = (xt @ re_w + re_b).astype(jnp.float32).reshape(-1, N_GROUPS, EXPERTS_PER_GROUP)
    elog_g = jnp.take_along_axis(elog, gsel[:, None, None], 1)[:, 0]
    top_val, top_idx = lax.top_k(elog_g, TOP_K)
    wts = jax.nn.softmax(top_val, -1) * p_g
    eid = (gsel[:, None] * EXPERTS_PER_GROUP + top_idx).reshape(-1)
    y = grouped_experts(xt, eid, wts.reshape(-1), w1, w3, w2)
    return y.reshape(b_, l_, d_)


def setup_inputs(seed: int = 0) -> dict:
    key = jax.random.key(seed)
    ks = iter(jax.random.split(key, 48))

    def nrm(shape, scale):
        return scale * jax.random.normal(next(ks), shape, jnp.float32)

    L = DEPTH
    lam_u = jax.random.uniform(next(ks), (L, 2, D_B), jnp.float32, 0.9, 0.999)
    s = lam_u ** (1.0 / LRU_C)
    return {
        'x': nrm((BATCH, SEQ, D_MODEL), 1.0),
        'c': nrm((BATCH, D_MODEL), 1.0),
        'ctx': nrm((BATCH, CTX_LEN, D_MODEL), 1.0),
        'c_ctx': nrm((D_MODEL,), 1.0),
        'w_ada': nrm((L, D_MODEL, 6 * D_MODEL), 0.5 * D_MODEL ** -0.5),
        'b_ada': nrm((L, 6 * D_MODEL), 0.01),
        'w_in': nrm((L, D_MODEL, D_IN), D_MODEL ** -0.5),
        'mu_a': jax.random.uniform(next(ks), (L, A_SLAB), jnp.float32),
        'w0': jax.random.uniform(next(ks), (L, 2, D_A), jnp.float32, -6.0, 1.0),
        'w2': nrm((L, 2, LORA_DECAY, D_A), 0.5 * LORA_DECAY ** -0.5),
        'a0': nrm((L, 2, D_A), 0.5),
        'a2': nrm((L, 2, LORA_AAA, D_A), 0.5 * LORA_AAA ** -0.5),
        'g2': nrm((L, LORA_GATE, D_A), LORA_GATE ** -0.5),
        'k_k': 0.85 + nrm((L, D_A), 0.02),
        'k_a': 1.0 + nrm((L, D_A), 0.02),
        'r_k': nrm((L, A_HEADS, A_HEAD_DIM), 0.1),
        'gn_g': 1.0 + nrm((L, D_A), 0.02),
        'gn_b': nrm((L, D_A), 0.01),
        'conv_w': nrm((L, CONV_W, D_B), CONV_W ** -0.5),
        'conv_b': nrm((L, D_B), 0.01),
        'lru_wa': nrm((L, 2, B_BLOCKS, B_BLOCK_DIM, B_BLOCK_DIM), B_BLOCK_DIM ** -0.5),
        'lru_ba': nrm((L, 2, D_B), 0.01),
        'lru_wx': nrm((L, 2, B_BLOCKS, B_BLOCK_DIM, B_BLOCK_DIM), B_BLOCK_DIM ** -0.5),
        'lru_bx': nrm((L, 2, D_B), 0.01),
        'lru_lam': jnp.log(s) - jnp.log1p(-s),
        'p_a': nrm((L, D_A, D_MODEL), BETA * D_A ** -0.5),
        'p_b': nrm((L, D_B, D_MODEL), BETA * D_B ** -0.5),
        'w_o': nrm((L, D_MODEL, D_MODEL), BETA * D_MODEL ** -0.5),
        'ln1_g': 1.0 + nrm((L, D_MODEL), 0.02),
        'ln1_b': nrm((L, D_MODEL), 0.01),
        'router_g': nrm((L, D_MODEL, N_GROUPS), D_MODEL ** -0.5),
        'router_g_b': nrm((L, N_GROUPS), 0.01),
        'router_e': nrm((L, D_MODEL, N_EXPERTS), D_MODEL ** -0.5),
        'router_e_b': nrm((L, N_EXPERTS), 0.01),
        'e_w1': nrm((L, N_EXPERTS, D_MODEL, D_EXPERT), D_MODEL ** -0.5),
        'e_w3': nrm((L, N_EXPERTS, D_MODEL, D_EXPERT), D_MODEL ** -0.5),
        'e_w2': nrm((L, N_EXPERTS, D_EXPERT, D_MODEL), BETA * D_EXPERT ** -0.5),
        'ln2_g': 1.0 + nrm((L, D_MODEL), 0.02),
        'ln2_b': nrm((L, D_MODEL), 0.01),
    }


def reference(x, c, ctx, c_ctx, w_ada, b_ada, w_in, mu_a, w0, w2, a0, a2, g2, k_k, k_a, r_k,
              gn_g, gn_b, conv_w, conv_b, lru_wa, lru_ba, lru_wx, lru_bx, lru_lam,
              p_a, p_b, w_o, ln1_g, ln1_b, router_g, router_g_b, router_e, router_e_b,
              e_w1, e_w3, e_w2, ln2_g, ln2_b):
    b_ = x.shape[0]
    x = layer_norm(x)
    ctx = layer_norm(ctx)
    for l in range(DEPTH):
        mod = jax.nn.silu(c) @ w_ada[l] + b_ada[l]
        sh1, sc1, gt1, sh2, sc2, gt2 = jnp.split(mod[:, None, :], 6, axis=-1)
        mod_c = jax.nn.silu(c_ctx) @ w_ada[l] + b_ada[l]
        csh1, csc1, cgt1, csh2, csc2, cgt2 = jnp.split(mod_c, 6)
        rw = (mu_a[l], w0[l], w2[l], a0[l], a2[l], g2[l], k_k[l], k_a[l], r_k[l], gn_g[l], gn_b[l])
        lru = (conv_w[l], conv_b[l], lru_wa[l], lru_ba[l], lru_wx[l], lru_bx[l], lru_lam[l])
        moe_p = (router_g[l], router_g_b[l], router_e[l], router_e_b[l], e_w1[l], e_w3[l], e_w2[l])
        s0 = jnp.zeros((b_, A_HEADS, A_HEAD_DIM, A_HEAD_DIM), x.dtype)
        h0 = jnp.zeros((b_, D_B), x.dtype)
        mix_c, ctx_states = token_mix(ctx * (1.0 + csc1) + csh1, seq_shift, w_in[l], p_a[l], p_b[l], w_o[l],
                                      rw, lru, (s0, s0, h0, h0))
        mix_x, _ = token_mix(x * (1.0 + sc1) + sh1, grid_shift, w_in[l], p_a[l], p_b[l], w_o[l],
                             rw, lru, ctx_states)
        x = layer_norm(ALPHA * x + gt1 * mix_x, ln1_g[l], ln1_b[l])
        x = layer_norm(ALPHA * x + gt2 * hier_moe(x * (1.0 + sc2) + sh2, *moe_p), ln2_g[l], ln2_b[l])
        if l < DEPTH - 1:
            ctx = layer_norm(ALPHA * ctx + cgt1 * mix_c, ln1_g[l], ln1_b[l])
            ctx = layer_norm(ALPHA * ctx + cgt2 * hier_moe(ctx * (1.0 + csc2) + csh2, *moe_p),
                             ln2_g[l], ln2_b[l])
    return x
```

```python
import numpy as np
import concourse.bass as bass
import concourse.mybir as mybir
from concourse.bass_utils import run_bass_kernel_spmd

F32 = mybir.dt.float32
BF16 = mybir.dt.bfloat16
I32 = mybir.dt.int32
U32 = mybir.dt.uint32
AF = mybir.ActivationFunctionType
ALU = mybir.AluOpType
AX = mybir.AxisListType


class Prog:
    ENGS = ("pe", "dve", "act", "pool", "sp")

    def __init__(self, nc, n_dma_sems=12):
        self.nc = nc
        self.ops = []
        self.cnt = {}
        self.known = {e: {} for e in self.ENGS}
        self.lastw = {}
        self.readers = {}
        self.n_dma_sems = n_dma_sems
        self.dma_rr = {"sp": 0, "pool": 0, "act": 0}
        self.pending = {e: {} for e in self.ENGS}

    def barrier(self):
        snap = dict(self.cnt)
        for e in self.ENGS:
            for sk, v in snap.items():
                if self.pending[e].get(sk, 0) < v:
                    self.pending[e][sk] = v

    def _add(self, eng, fn, reads, writes, semkey, incv):
        deps = []
        for r in reads:
            if r in self.lastw:
                deps.append(self.lastw[r])
        for w in writes:
            if w in self.lastw:
                deps.append(self.lastw[w])
            deps.extend(self.readers.get(w, ()))
        waits = {}
        kn = self.known[eng]
        if self.pending[eng]:
            deps = deps + list(self.pending[eng].items())
            self.pending[eng] = {}
        for (sk, v) in deps:
            if eng == "pe" and sk == ("e", "pe"):
                continue
            if kn.get(sk, 0) >= v:
                continue
            if waits.get(sk, 0) < v:
                waits[sk] = v
        for sk, v in waits.items():
            kn[sk] = v
        self.cnt[semkey] = self.cnt.get(semkey, 0) + incv
        me = (semkey, self.cnt[semkey])
        self.ops.append((eng, fn, list(waits.items()), semkey, incv))
        for r in reads:
            self.readers.setdefault(r, []).append(me)
        for w in writes:
            self.lastw[w] = me
            self.readers[w] = []
        return me

    def op(self, eng, fn, reads=(), writes=()):
        return self._add(eng, fn, reads, writes, ("e", eng), 1)

    def dma(self, q, fn, reads=(), writes=()):
        i = self.dma_rr[q]
        self.dma_rr[q] = (i + 1) % self.n_dma_sems
        return self._add(q, fn, reads, writes, ("d", q, i), 16)

    def flush(self, final=False, final_wait_eng="sp"):
        import contextlib
        nc = self.nc
        if not hasattr(self, "_semstack"):
            self._semstack = contextlib.ExitStack()
            self._sems = {}
            self._emitted = 0
        for sk in sorted(self.cnt.keys(), key=str):
            if sk not in self._sems:
                self._sems[sk] = self._semstack.enter_context(nc.semaphore("s_" + "_".join(str(x) for x in sk)))
        sems = self._sems
        ops = self.ops[self._emitted:]
        self._emitted = len(self.ops)
        per = {e: [] for e in self.ENGS}
        for o in ops:
            per[o[0]].append(o)
        finals = [(sk, v) for sk, v in self.cnt.items()] if final else []
        with nc.Block() as block:
            def mk(engname):
                def body(eng):
                    for (_, fn, waits, sk, incv) in per[engname]:
                        for (wsk, v) in waits:
                            eng.wait_ge(sems[wsk], v)
                        ins = fn(eng)
                        ins.then_inc(sems[sk], incv)
                    if engname == final_wait_eng:
                        for (wsk, v) in finals:
                            eng.wait_ge(sems[wsk], v)
                return body
            block.tensor(mk("pe"))
            block.vector(mk("dve"))
            block.scalar(mk("act"))
            block.gpsimd(mk("pool"))
            block.sync(mk("sp"))
        if final:
            self._semstack.close()

    def emit(self, final_wait_eng="sp"):
        self.flush(final=True, final_wait_eng=final_wait_eng)


def _apname(v):
    try:
        return v.tensor.name
    except Exception:
        return v.name


def _is_ap(v):
    return hasattr(v, "ap") and hasattr(v, "dtype") and hasattr(v, "shape") and not isinstance(v, (int, float))


WRITE_KW = ("out", "out_max", "out_indices", "accum_out")


def I(P, eng, method, *args, _w=None, _r=None, _xr=(), _xw=(), **kw):
    reads, writes = [], []
    for k, v in kw.items():
        if _is_ap(v):
            (writes if k in WRITE_KW else reads).append(_apname(v))
    if method == "matmul" or method == "transpose":
        if args and _is_ap(args[0]):
            writes.append(_apname(args[0]))
        if method == "matmul" and kw.get("start", True) is False:
            reads.append(writes[-1])
    if _w is not None:
        writes = list(_w)
    if _r is not None:
        reads = list(_r)
    reads += list(_xr)
    writes += list(_xw)
    fn = lambda e: getattr(e, method)(*args, **kw)
    if method == "dma_start":
        return P.dma(eng, fn, reads=reads, writes=writes)
    return P.op(eng, fn, reads=reads, writes=writes)


import contextlib

A_SLAB = 3360
NT = 19
LAT0 = 256
NLAT = 8192
NTOK = LAT0 + NLAT
CV = {}
_o = 0
for nm, n in [("mu", NT), ("msk", NT * 6), ("w0", 4), ("a0", 4), ("kk", 4), ("ka", 4), ("rk", 4),
              ("cw", 20), ("cb", 4), ("lba", 4), ("lbx", 4), ("lam", 4), ("bada", 16)]:
    CV[nm] = _o; _o += n
NCV = _o
C_ID, C_SU, C_UI, C_SL, C_OB, C_HS, C_RST = 0, 128, 256, 384, 512, 640, 642
NCST = 642 + 512


def host_consts():
    c = np.zeros((128, NCST), np.float32)
    c[:, C_ID:C_ID + 128] = np.eye(128)
    blk = np.zeros((128, 128), np.float32); blk[:64, :64] = 1; blk[64:, 64:] = 1
    i = np.arange(128)[:, None] % 64; j = np.arange(128)[None, :] % 64
    c[:, C_SU:C_SU + 128] = blk * (i < j)
    c[:, C_UI:C_UI + 128] = blk * (i <= j)
    c[:, C_SL:C_SL + 128] = blk * (i > j)
    c[:, C_OB:C_OB + 128] = blk
    c[:64, C_HS] = 1; c[64:, C_HS + 1] = 1
    r = np.ones(512, np.float32); r[::64] = 0
    c[:, C_RST:C_RST + 512] = r[None]
    return c


def prep1(inp, b, e, hh):
    x = inp["x"][b]; ctx = inp["ctx"][b]
    if e == 1:
        x = x[::-1]; ctx = ctx[::-1]
    x1 = np.ascontiguousarray(np.concatenate([ctx, x], 0))
    w_in = inp["w_in"][0]
    ch = np.arange(512 * hh, 512 * hh + 512)
    cols = np.full((NT, 128), -1, np.int64)
    for t in range(4):
        cols[t] = ch[128 * t:128 * t + 128]
        cols[4 + t] = 1024 + ch[128 * t:128 * t + 128]
        cols[8 + t] = 2048 + ch[128 * t:128 * t + 128]
        cols[15 + t] = A_SLAB + ch[128 * t:128 * t + 128]
    cols[12] = np.arange(3072, 3200)
    cols[13] = np.arange(3200, 3328)
    cols[14, :32] = np.arange(3328, 3360)
    w1 = np.zeros((NT, 128, 8, 128), np.float32)
    wk = w_in.reshape(8, 128, -1)
    for t in range(NT):
        valid = cols[t] >= 0
        w1[t][:, :, valid] = np.transpose(wk[:, :, cols[t][valid]], (1, 0, 2))
    cv = np.zeros((128, NCV), np.float32)
    mu_a = inp["mu_a"][0]
    for t in range(NT):
        for p in range(128):
            c = cols[t, p]
            if 0 <= c < A_SLAB:
                cv[p, CV["mu"] + t] = mu_a[c]
                d = c % 4
                off = {0: 0, 1: 1, 2: 2, 3: 3}[d]
                if e == 1:
                    off = {0: 1, 1: 0, 2: 3, 3: 2}[off]
                cv[p, CV["msk"] + t * 6 + off] = 1
                offc = 0 if c % 2 == 0 else 1
                if e == 1:
                    offc = 1 - offc
                cv[p, CV["msk"] + t * 6 + 4 + offc] = 1
    def fm(vec512):
        return vec512.reshape(4, 128).T
    cv[:, CV["w0"]:CV["w0"] + 4] = fm(inp["w0"][0, e][ch])
    cv[:, CV["a0"]:CV["a0"] + 4] = fm(inp["a0"][0, e][ch])
    cv[:, CV["kk"]:CV["kk"] + 4] = fm(inp["k_k"][0][ch])
    cv[:, CV["ka"]:CV["ka"] + 4] = fm(inp["k_a"][0][ch])
    cv[:, CV["rk"]:CV["rk"] + 4] = fm(inp["r_k"][0].reshape(-1)[ch])
    cw = inp["conv_w"][0]
    if e == 1:
        cw = cw[::-1]
    for t in range(4):
        cv[:, CV["cw"] + 5 * t:CV["cw"] + 5 * t + 5] = cw[:, ch[128 * t:128 * t + 128]].T
    cv[:, CV["cb"]:CV["cb"] + 4] = fm(inp["conv_b"][0][ch])
    cv[:, CV["lba"]:CV["lba"] + 4] = fm(inp["lru_ba"][0, e][ch])
    cv[:, CV["lbx"]:CV["lbx"] + 4] = fm(inp["lru_bx"][0, e][ch])
    cv[:, CV["lam"]:CV["lam"] + 4] = fm(inp["lru_lam"][0, e][ch])
    cv[:, CV["bada"]:CV["bada"] + 16] = inp["b_ada"][0][:2048].reshape(16, 128).T
    lora = np.zeros((128, 4, 512), np.float32)
    lora[:64, 0] = inp["w2"][0, e][:, ch]
    lora[64:, 1] = inp["a2"][0, e][:, ch]
    lora[:, 2] = inp["g2"][0][:128, ch]
    lora[:32, 3] = inp["g2"][0][128:160, ch]
    lruw = np.zeros((128, 8, 128), np.float32)
    for t in range(4):
        for s in range(2):
            blk = 8 * hh + 2 * t + s
            lruw[64 * s:64 * s + 64, t, 64 * s:64 * s + 64] = inp["lru_wa"][0, e, blk]
            lruw[64 * s:64 * s + 64, 4 + t, 64 * s:64 * s + 64] = inp["lru_wx"][0, e, blk]
    cT = np.stack([inp["c"][b].reshape(8, 128).T, inp["c_ctx"].reshape(8, 128).T], -1)
    wada = np.ascontiguousarray(np.transpose(inp["w_ada"][0][:, :2048].reshape(8, 128, 2048), (1, 0, 2)))
    return dict(x1=x1, w1=w1, cv=cv, lora=lora, lruw=lruw, cT=np.ascontiguousarray(cT), wada=wada, cst=host_consts())


def build_p1(nc, n_lat_sb=16, debug=False, stop=None):
    P = Prog(nc)
    def din(name, shape):
        return nc.dram_tensor(name, shape, F32, kind="ExternalInput").ap()
    def dout(name, shape):
        return nc.dram_tensor(name, shape, F32, kind="ExternalOutput").ap()
    x1 = din("x1", [NTOK, 1024]); w1 = din("w1", [NT, 128, 8, 128]); cvd = din("cv", [128, NCV])
    lorad = din("lora", [128, 4, 512]); lruwd = din("lruw", [128, 8, 128]); cTd = din("cT", [128, 8, 2])
    wadad = din("wada", [128, 8, 2048]); cstd = din("cst", [128, NCST])
    yout = dout("yout", [NLAT, 512]); vout = dout("vout", [NLAT, 512]); sout = dout("sout", [NLAT, 8])
    gout = dout("gout", [NLAT, 512]); hout = dout("hout", [512, NLAT])
    st = contextlib.ExitStack()
    with st:
        def sb(name, shape, dt=F32):
            return st.enter_context(nc.sbuf_tensor("sb_" + name, shape, dt))
        def ps(name, shape, dt=F32):
            return st.enter_context(nc.psum_tensor("pp_" + name, shape, dt))
        cst = sb("cst", [128, NCST]); cstb = sb("cstb", [128, 640], BF16)
        cv = sb("cvs", [128, NCV]); drv = sb("drv", [128, NT * 7 + 8])
        modv = sb("modv", [128, 16, 2]); scp = sb("scp", [128, 8, 2])
        cTs = sb("cTs", [128, 8, 2]); cTa = sb("cTa", [128, 8, 2])
        wap = sb("wap", [128, 8, 128])
        w1b = [sb(f"w1b{i}", [128, 8, 128], BF16) for i in range(3)]
        lorab = sb("lorab", [128, 4, 512], BF16); lruwb = sb("lruwb", [128, 8, 128], BF16)
        xt = [sb(f"xt{i}", [128, 1024]) for i in range(2)]
        stats = sb("stats", [128, 2, 6]); mv = sb("mv", [128, 2]); rstd = sb("rstd", [128, 1])
        xn = sb("xn", [128, 1024])
        hT = sb("hT", [128, 8, 640], BF16)
        ssb = [sb(f"ssb{i}", [128, 640]) for i in range(2)]
        za = [sb(f"za{i}", [128, 512]) for i in range(15)]
        tmpn = ["sg", "aa", "km", "kr", "sq", "kn", "bb", "cl", "e1", "e2", "e3", "e4", "t1"]
        T = {n: sb("t_" + n, [128, 512]) for n in tmpn}
        act12 = sb("act12", [128, 512], BF16); sgd13 = sb("sgd13", [128, 512], BF16); sgd14 = sb("sgd14", [32, 512], BF16)
        ARx = [sb(f"ARx{i}", [128, 8, 2, 128], BF16) for i in range(2)]
        BTx = [sb(f"BTx{i}", [128, 8, 128], BF16) for i in range(2)]
        KTx = [sb(f"KTx{i}", [128, 8, 128], BF16) for i in range(2)]
        BDx = [sb(f"BDx{i}", [128, 8, 128], BF16) for i in range(2)]
        KDx = [sb(f"KDx{i}", [128, 8, 128], BF16) for i in range(2)]
        BDT = [sb(f"BDT{i}", [128, 8, 128], BF16) for i in range(2)]
        KDT = [sb(f"KDT{i}", [128, 8, 128], BF16) for i in range(2)]
        Vx = sb("Vx", [128, 8, 128])
        Vst = [sb(f"Vst{i}", [128, 8, 128]) for i in range(2)]
        Vb = [sb(f"Vb{i}", [128, 8, 128], BF16) for i in range(2)]
        Yst = [sb(f"Yst{i}", [128, 8, 128]) for i in range(2)]
        gC = [sb(f"gC{i}", [128, 8]) for i in range(2)]
        Zf = [sb(f"Zf{i}", [128, 128]) for i in range(4)]
        Zb = [sb(f"Zb{i}", [128, 128], BF16) for i in range(4)]
        cw_ = {}
        for cp in range(2):
            for n in ["U0A", "AK"]:
                cw_[n, cp] = sb(f"c_{n}{cp}", [128, 256], BF16)
            for n in ["N0", "N1", "U1", "N2", "U2", "MT0", "MT1", "X", "Pb"]:
                cw_[n, cp] = sb(f"c_{n}{cp}", [128, 128], BF16)
        gst = sb("gst", [128, 4, 512]); sst = sb("sst", [128, 4, 8])
        xb = sb("l_xb", [128, 512]); xbb = sb("l_xbb", [128, 512], BF16)
        L = {n: sb("l_" + n, [128, 512]) for n in ["rg", "ig", "la", "a2", "uu"]}
        hb = [sb(f"hbuf{i}", [128, 512]) for i in range(4)]
        hlast = sb("hlast", [128, 4])
        ps_s0 = ps("ps_s0", [128, 512]); ps_s1 = ps("ps_s1", [128, 512])
        ps_l = ps("ps_l", [128, 512]); ps_l2 = ps("ps_l2", [128, 512])
        ps_w0 = ps("ps_w0", [128, 512]); ps_w1 = ps("ps_w1", [128, 512]); ps_w2 = ps("ps_w2", [128, 512])
        pst = ps("pst", [128, 1024], BF16)

        ps_inv = ps_s1
        PW1 = ["pw1_0", "pw1_1", "pw1_2", "pw1_3"]
        ident = cst[:, C_ID:C_ID + 128]
        identb = cstb[:, 0:128]
        M1b = cst[:, C_SU:C_SU + 256]
        SLb = cst[:, C_SL:C_SL + 128]
        OBf = cst[:, C_OB:C_OB + 128]
        rstm = cst[:, C_RST:C_RST + 512]
        hsel = cst[:, C_HS:C_HS + 2]

        I(P, "sp", "dma_start", out=cst[:], in_=cstd)
        I(P, "sp", "dma_start", out=cv[:], in_=cvd)
        I(P, "sp", "dma_start", out=cTs[:], in_=cTd)
        I(P, "pool", "dma_start", out=lorab[:], in_=lorad)
        I(P, "pool", "dma_start", out=lruwb[:], in_=lruwd)
        I(P, "dve", "tensor_copy", out=cstb[:, 0:128], in_=cst[:, C_ID:C_ID + 128])
        I(P, "dve", "tensor_copy", out=cstb[:, 128:512], in_=cst[:, C_SU:C_SU + 384])
        I(P, "dve", "tensor_copy", out=cstb[:, 512:640], in_=cst[:, C_OB:C_OB + 128])
        omm = drv[:, 0:NT]; coef = drv[:, NT:NT * 7]; omka = drv[:, NT * 7:NT * 7 + 4]; cA = drv[:, NT * 7 + 4:NT * 7 + 8]
        I(P, "dve", "tensor_scalar", out=omm, in0=cv[:, CV["mu"]:CV["mu"] + NT], scalar1=-1.0, scalar2=1.0, op0=ALU.mult, op1=ALU.add)
        I(P, "dve", "tensor_tensor", out=drv[:, NT:NT * 7].rearrange("p (t s) -> p t s", s=6),
          in0=cv[:, CV["msk"]:CV["msk"] + NT * 6].rearrange("p (t s) -> p t s", s=6),
          in1=cv[:, CV["mu"]:CV["mu"] + NT].unsqueeze(2).to_broadcast([128, NT, 6]), op=ALU.mult)
        I(P, "dve", "tensor_scalar", out=omka, in0=cv[:, CV["ka"]:CV["ka"] + 4], scalar1=-1.0, scalar2=1.0, op0=ALU.mult, op1=ALU.add)
        I(P, "act", "activation", out=cA, in_=cv[:, CV["lam"]:CV["lam"] + 4], func=AF.Exp, scale=-1.0)
        I(P, "act", "activation", out=cA, in_=cA, func=AF.Ln, bias=1.0)
        I(P, "dve", "tensor_scalar", out=cA, in0=cA, scalar1=-8.0, scalar2=None, op0=ALU.mult)
        I(P, "act", "activation", out=cTa[:], in_=cTs[:], func=AF.Silu)
        for j in range(16):
            I(P, "sp", "dma_start", out=wap[:], in_=wadad[:, :, j * 128:(j + 1) * 128])
            for kc in range(8):
                I(P, "pe", "matmul", ps_l[:, 2 * j:2 * j + 2], lhsT=wap[:, kc, :], rhs=cTa[:, kc, :], start=(kc == 0), stop=(kc == 7))
        I(P, "dve", "tensor_tensor", out=modv[:], in0=ps_l[:, 0:32].rearrange("p (j n) -> p j n", n=2),
          in1=cv[:, CV["bada"]:CV["bada"] + 16].unsqueeze(2).to_broadcast([128, 16, 2]), op=ALU.add)
        I(P, "dve", "tensor_scalar", out=scp[:], in0=modv[:, 8:16, :], scalar1=1.0, scalar2=None, op0=ALU.add)
        for i in range(2):
            for tns in (ARx[i], BTx[i], KTx[i], BDx[i], KDx[i]):
                I(P, "pool", "memset", tns[:], 0.0, _w=[tns.name])
        I(P, "pool", "memset", Vx[:], 0.0, _w=[Vx.name])
        for i in range(4):
            I(P, "pool", "memset", Zf[i][:], 0.0, _w=[Zf[i].name])
            I(P, "pool", "memset", Zb[i][:], 0.0, _w=[Zb[i].name])
        for i in range(2):
            I(P, "pool", "memset", xt[i][:], 0.0, _w=[xt[i].name])

        sbs = [dict(row0=0, n=256, hl=0, ctx=True, first=False, last=False, lat0=None)]
        for i in range(n_lat_sb):
            sbs.append(dict(row0=LAT0 + 512 * i - 64, n=512, hl=64, ctx=False, first=(i == 0), last=(i == NLAT // 512 - 1), lat0=512 * i))
        w1i = [0]
        xti = [0]
        hprev = [None] * 4
        for sbi, S in enumerate(sbs):
            if stop == 'setup': break
            n, hl = S["n"], S["hl"]; W = n + 2 * hl; nch = n // 64
            mcol = 1 if S["ctx"] else 0
            ntile = W // 128
            for ti in range(ntile):
                r0 = S["row0"] + ti * 128
                lo, hi = 0, 128
                if r0 < 0: lo = -r0
                if r0 + 128 > NTOK: hi = NTOK - r0
                xb_ = xt[xti[0] % 2]; xti[0] += 1
                I(P, "sp", "dma_start", out=xb_[lo:hi, :], in_=x1[r0 + lo:r0 + hi, :])
                for hf in range(2):
                    I(P, "dve", "bn_stats", out=stats[:, hf, :], in_=xb_[:, hf * 512:(hf + 1) * 512])
                I(P, "dve", "bn_aggr", out=mv[:], in_=stats[:])
                I(P, "act", "activation", out=rstd[:], in_=mv[:, 1:2], func=AF.Sqrt, bias=1e-5)
                I(P, "dve", "reciprocal", out=rstd[:], in_=rstd[:])
                I(P, "dve", "tensor_scalar", out=xn[:], in0=xb_[:], scalar1=mv[:, 0:1], scalar2=rstd[:, 0:1], op0=ALU.subtract, op1=ALU.mult)
                for kc in range(8):
                    pso = (ps_s0 if kc < 4 else ps_s1)[:, (kc % 4) * 128:(kc % 4) * 128 + 128]
                    I(P, "pe", "transpose", pso, in_=xn[:, kc * 128:(kc + 1) * 128], identity=ident, _r=[xn.name, cst.name])
                for kc in range(8):
                    pso = (ps_s0 if kc < 4 else ps_s1)[:, (kc % 4) * 128:(kc % 4) * 128 + 128]
                    if kc % 2 == 0:
                        I(P, "dve", "tensor_scalar", out=hT[:, kc, ti * 128:(ti + 1) * 128], in0=pso, scalar1=scp[:, kc, mcol:mcol + 1],
                          scalar2=modv[:, kc, mcol:mcol + 1], op0=ALU.mult, op1=ALU.add)
                    else:
                        I(P, "act", "activation", out=hT[:, kc, ti * 128:(ti + 1) * 128], in_=pso, func=AF.Identity,
                          scale=scp[:, kc, mcol:mcol + 1], bias=modv[:, kc, mcol:mcol + 1])
            if S["first"]:
                I(P, "pool", "memset", hT[:, :, 0:64], 0.0, _w=[hT.name])
            if S["last"]:
                I(P, "pool", "memset", hT[:, :, W - 64:W], 0.0, _w=[hT.name])
            if stop == 'a': continue
            for ct in range(NT):
                wb = w1b[w1i[0] % 3]; w1i[0] += 1
                I(P, "pool", "dma_start", out=wb[:], in_=w1[ct])
                M = 32 if ct == 14 else 128
                regions = [(0, min(W, 512), ps_s0)] + ([(512, W, ps_s1)] if W > 512 else [])
                for (a0_, a1_, pt) in regions:
                    for kc in range(8):
                        I(P, "pe", "matmul", pt[0:M, 0:a1_ - a0_], lhsT=wb[:, kc, 0:M], rhs=hT[:, kc, a0_:a1_], start=(kc == 0), stop=(kc == 7))
                s_ = ssb[ct % 2]
                for (a0_, a1_, pt) in regions:
                    I(P, "act", "copy", out=s_[0:M, a0_:a1_], in_=pt[0:M, 0:a1_ - a0_])
                if ct < 15:
                    z = za[ct]
                    I(P, "dve", "tensor_scalar", out=z[0:M, 0:n], in0=s_[0:M, hl:hl + n], scalar1=omm[0:M, ct:ct + 1], scalar2=None, op0=ALU.mult)
                    def cf(d):
                        return coef[0:M, ct * 6 + d:ct * 6 + d + 1]
                    if S["ctx"]:
                        I(P, "dve", "scalar_tensor_tensor", out=z[0:M, 1:n], in0=s_[0:M, 0:n - 1], scalar=cf(4), in1=z[0:M, 1:n], op0=ALU.mult, op1=ALU.add)
                        I(P, "dve", "scalar_tensor_tensor", out=z[0:M, 0:n - 1], in0=s_[0:M, 1:n], scalar=cf(5), in1=z[0:M, 0:n - 1], op0=ALU.mult, op1=ALU.add)
                    else:
                        zv = z[0:M, 0:n].rearrange("p (r c) -> p r c", c=64)
                        sv = s_[0:M, hl:hl + n].rearrange("p (r c) -> p r c", c=64)
                        I(P, "dve", "scalar_tensor_tensor", out=zv[:, :, 1:64], in0=sv[:, :, 0:63], scalar=cf(0), in1=zv[:, :, 1:64], op0=ALU.mult, op1=ALU.add)
                        I(P, "dve", "scalar_tensor_tensor", out=zv[:, :, 0:63], in0=sv[:, :, 1:64], scalar=cf(1), in1=zv[:, :, 0:63], op0=ALU.mult, op1=ALU.add)
                        I(P, "dve", "scalar_tensor_tensor", out=z[0:M, 0:n], in0=s_[0:M, 0:n], scalar=cf(2), in1=z[0:M, 0:n], op0=ALU.mult, op1=ALU.add)
                        I(P, "dve", "scalar_tensor_tensor", out=z[0:M, 0:n], in0=s_[0:M, 2 * hl:2 * hl + n], scalar=cf(3), in1=z[0:M, 0:n], op0=ALU.mult, op1=ALU.add)
                else:
                    lt = ct - 15
                    cwo = CV["cw"] + 5 * lt
                    I(P, "dve", "tensor_scalar", out=xb[:, 0:n], in0=s_[:, hl:hl + n], scalar1=cv[:, cwo + 2:cwo + 3], scalar2=cv[:, CV["cb"] + lt:CV["cb"] + lt + 1], op0=ALU.mult, op1=ALU.add)
                    for j in (0, 1, 3, 4):
                        d = j - 2
                        if hl > 0:
                            o0, o1, i0 = 0, n, hl + d
                        else:
                            o0, o1 = max(0, -d), n - max(0, d); i0 = o0 + d
                        I(P, "dve", "scalar_tensor_tensor", out=xb[:, o0:o1], in0=s_[:, i0:i0 + (o1 - o0)], scalar=cv[:, cwo + j:cwo + j + 1], in1=xb[:, o0:o1], op0=ALU.mult, op1=ALU.add)
                    I(P, "act", "copy", out=xbb[:, 0:n], in_=xb[:, 0:n])
                    I(P, "pe", "matmul", ps_l[:, 0:n], lhsT=lruwb[:, lt, :], rhs=xbb[:, 0:n], start=True, stop=True)
                    I(P, "pe", "matmul", ps_l2[:, 0:n], lhsT=lruwb[:, 4 + lt, :], rhs=xbb[:, 0:n], start=True, stop=True)
                    I(P, "act", "activation", out=L["rg"][:, 0:n], in_=ps_l[:, 0:n], func=AF.Sigmoid, bias=cv[:, CV["lba"] + lt:CV["lba"] + lt + 1])
                    I(P, "act", "activation", out=L["ig"][:, 0:n], in_=ps_l2[:, 0:n], func=AF.Sigmoid, bias=cv[:, CV["lbx"] + lt:CV["lbx"] + lt + 1])
                    I(P, "act", "activation", out=L["la"][:, 0:n], in_=L["rg"][:, 0:n], func=AF.Exp, scale=cA[:, lt:lt + 1])
                    I(P, "dve", "tensor_tensor", out=L["a2"][:, 0:n], in0=L["la"][:, 0:n], in1=L["la"][:, 0:n], op=ALU.mult)
                    I(P, "dve", "tensor_scalar", out=L["a2"][:, 0:n], in0=L["a2"][:, 0:n], scalar1=-1.0, scalar2=1.0, op0=ALU.mult, op1=ALU.add)
                    I(P, "act", "activation", out=L["a2"][:, 0:n], in_=L["a2"][:, 0:n], func=AF.Sqrt)
                    I(P, "dve", "tensor_tensor", out=L["uu"][:, 0:n], in0=L["ig"][:, 0:n], in1=xb[:, 0:n], op=ALU.mult)
                    I(P, "dve", "tensor_tensor", out=L["uu"][:, 0:n], in0=L["uu"][:, 0:n], in1=L["a2"][:, 0:n], op=ALU.mult)
                    hcur = hb[lt]
                    init = 0.0 if hprev[lt] is None else hprev[lt]
                    kwx = {} if hprev[lt] is None else dict(_xr=[hlast.name])
                    I(P, "dve", "tensor_tensor_scan", out=hcur[:, 0:n], data0=L["la"][:, 0:n], data1=L["uu"][:, 0:n], initial=init, op0=ALU.mult, op1=ALU.add, **kwx)
                    I(P, "dve", "tensor_copy", out=hlast[:, lt:lt + 1], in_=hcur[:, n - 1:n])
                    hprev[lt] = hlast[:, lt:lt + 1]
                    if not S["ctx"]:
                        I(P, "sp", "dma_start", out=hout[lt * 128:(lt + 1) * 128, S["lat0"]:S["lat0"] + n], in_=hcur[:, 0:n])
            if stop == 'b': continue
            I(P, "act", "activation", out=act12[0:64, 0:n], in_=za[12][0:64, 0:n], func=AF.Tanh)
            I(P, "act", "copy", out=act12[64:128, 0:n], in_=za[12][64:128, 0:n])
            I(P, "act", "activation", out=sgd13[:, 0:n], in_=za[13][:, 0:n], func=AF.Sigmoid)
            I(P, "act", "activation", out=sgd14[:, 0:n], in_=za[14][0:32, 0:n], func=AF.Sigmoid)
            if not S["ctx"]:
                for ts in range(n // 128):
                    I(P, "pe", "matmul", ps_l2[:, :], lhsT=sgd13[:, ts * 128:(ts + 1) * 128], rhs=lorab[:, 2, :], start=True, stop=False)
                    I(P, "pe", "matmul", ps_l2[:, :], lhsT=sgd14[:, ts * 128:(ts + 1) * 128], rhs=lorab[0:32, 3, :], start=False, stop=True)
                    I(P, "act", "copy", out=gst[:, ts, :], in_=ps_l2[:, :])
                I(P, "sp", "dma_start", out=gout[S["lat0"]:S["lat0"] + n, :].rearrange("(s p) c -> p s c", p=128), in_=gst[:, 0:n // 128, :])
            if stop == 'c': continue
            for t in range(4):
                tp = t % 2
                R_, K_, V_ = za[t], za[4 + t], za[8 + t]
                sl = slice(0, n)
                def col(nm):
                    return cv[:, CV[nm] + t:CV[nm] + t + 1]
                I(P, "pe", "matmul", ps_l[:, sl], lhsT=lorab[:, 0, t * 128:(t + 1) * 128], rhs=act12[:, sl], start=True, stop=True)
                I(P, "act", "activation", out=T["sg"][:, sl], in_=ps_l[:, sl], func=AF.Sigmoid, bias=col("w0"))
                I(P, "pe", "matmul", ps_l[:, sl], lhsT=lorab[:, 1, t * 128:(t + 1) * 128], rhs=act12[:, sl], start=True, stop=True)
                I(P, "act", "activation", out=T["aa"][:, sl], in_=ps_l[:, sl], func=AF.Sigmoid, bias=col("a0"))
                I(P, "dve", "tensor_scalar", out=T["sg"][:, sl], in0=T["sg"][:, sl], scalar1=-0.6065306597126334, scalar2=None, op0=ALU.mult)
                I(P, "dve", "tensor_tensor_scan", out=T["cl"][:, sl], data0=rstm[:, sl], data1=T["sg"][:, sl], initial=0.0, op0=ALU.mult, op1=ALU.add)
                I(P, "dve", "tensor_scalar", out=T["t1"][:, sl], in0=T["aa"][:, sl], scalar1=col("ka"), scalar2=omka[:, t:t + 1], op0=ALU.mult, op1=ALU.add)
                I(P, "dve", "tensor_tensor", out=T["km"][:, sl], in0=K_[:, sl], in1=T["t1"][:, sl], op=ALU.mult)
                I(P, "dve", "tensor_scalar", out=T["kr"][:, sl], in0=K_[:, sl], scalar1=col("kk"), scalar2=None, op0=ALU.mult)
                I(P, "act", "activation", out=T["sq"][:, sl], in_=T["kr"][:, sl], func=AF.Square)
                I(P, "pe", "matmul", ps_l[:, sl], lhsT=OBf, rhs=T["sq"][:, sl], start=True, stop=True)
                I(P, "act", "activation", out=T["sq"][:, sl], in_=ps_l[:, sl], func=AF.Sqrt, bias=1e-12)
                I(P, "dve", "reciprocal", out=T["sq"][:, sl], in_=T["sq"][:, sl])
                I(P, "dve", "tensor_tensor", out=T["kn"][:, sl], in0=T["kr"][:, sl], in1=T["sq"][:, sl], op=ALU.mult)
                I(P, "dve", "tensor_tensor", out=T["bb"][:, sl], in0=T["kn"][:, sl], in1=T["aa"][:, sl], op=ALU.mult)
                I(P, "act", "activation", out=T["e1"][:, sl], in_=T["cl"][:, sl], func=AF.Exp)
                I(P, "act", "activation", out=T["e2"][:, sl], in_=T["cl"][:, sl], func=AF.Exp, scale=-1.0)
                I(P, "dve", "tensor_tensor", out=T["e3"][:, sl], in0=T["cl"][:, sl], in1=T["sg"][:, sl], op=ALU.subtract)
                I(P, "act", "activation", out=T["e3"][:, sl], in_=T["e3"][:, sl], func=AF.Exp)
                clv = T["cl"][:, sl].rearrange("p (c j) -> p c j", j=64)
                I(P, "dve", "tensor_tensor", out=T["e4"][:, sl].rearrange("p (c j) -> p c j", j=64),
                  in0=clv[:, :, 63:64].to_broadcast([128, nch, 64]), in1=clv, op=ALU.subtract)
                I(P, "act", "activation", out=T["e4"][:, sl], in_=T["e4"][:, sl], func=AF.Exp)
                e1v = T["e1"][:, sl].rearrange("p (c j) -> p c j", j=64)
                I(P, "dve", "tensor_copy", out=gC[tp][:, 0:nch], in_=e1v[:, :, 63])
                if not S["ctx"]:
                    I(P, "dve", "scalar_tensor_tensor", out=T["t1"][:, sl], in0=R_[:, sl], scalar=col("rk"), in1=T["km"][:, sl], op0=ALU.mult, op1=ALU.mult)
                    for ts in range(n // 128):
                        I(P, "pe", "matmul", ps_l2[:, 2 * ts:2 * ts + 2], lhsT=T["t1"][:, ts * 128:(ts + 1) * 128], rhs=hsel, start=True, stop=True)
                    I(P, "dve", "tensor_copy", out=sst[:, :, 2 * t:2 * t + 2], in_=ps_l2[:, 0:8].rearrange("p (s h) -> p s h", h=2))
                def v3(ap, h):
                    return ap[64 * h:64 * h + 64, sl].rearrange("p (c j) -> p c j", j=64)
                for h in range(2):
                    hs = slice(64 * h, 64 * h + 64)
                    I(P, "dve", "scalar_tensor_tensor", out=ARx[tp][hs, 0:nch, 0, hs], in0=v3(T["kn"], h), scalar=-1.0, in1=v3(T["e3"], h), op0=ALU.mult, op1=ALU.mult)
                    I(P, "dve", "tensor_tensor", out=ARx[tp][hs, 0:nch, 1, hs], in0=v3(R_, h), in1=v3(T["e1"], h), op=ALU.mult)
                    I(P, "pool", "tensor_tensor", out=BTx[tp][hs, 0:nch, hs], in0=v3(T["bb"], h), in1=v3(T["e2"], h), op=ALU.mult)
                    I(P, "pool", "tensor_tensor", out=KTx[tp][hs, 0:nch, hs], in0=v3(T["km"], h), in1=v3(T["e2"], h), op=ALU.mult)
                    I(P, "pool", "tensor_tensor", out=BDx[tp][hs, 0:nch, hs], in0=v3(T["bb"], h), in1=v3(T["e4"], h), op=ALU.mult)
                    I(P, "pool", "tensor_tensor", out=KDx[tp][hs, 0:nch, hs], in0=v3(T["km"], h), in1=v3(T["e4"], h), op=ALU.mult)
                    I(P, "pool", "tensor_copy", out=Vx[hs, 0:nch, hs], in_=v3(V_, h))
                for c0 in range(0, nch, 4):
                    for c in range(c0, c0 + 4):
                        I(P, "pe", "transpose", ps_l[:, (c - c0) * 128:(c - c0 + 1) * 128], in_=Vx[:, c, :], identity=ident, _r=[Vx.name, cst.name])
                    I(P, "act", "copy", out=Vst[tp][:, c0:c0 + 4, :], in_=ps_l[:, :].rearrange("p (c j) -> p c j", j=128))
                I(P, "pool", "tensor_copy", out=Vb[tp][:, 0:nch, :], in_=Vst[tp][:, 0:nch, :])
                for (src, dst) in ((BDx, BDT), (KDx, KDT)):
                    for c in range(nch):
                        I(P, "pe", "transpose", pst[:, c * 128:(c + 1) * 128], in_=src[tp][:, c, :], identity=identb, _r=[src[tp].name, cstb.name])
                    I(P, "dve", "tensor_copy", out=dst[tp][:, 0:nch, :], in_=pst[:, 0:nch * 128].rearrange("p (c j) -> p c j", j=128))
                if not S["ctx"]:
                    for h in range(2):
                        hs = slice(64 * h, 64 * h + 64)
                        I(P, "sp", "dma_start", out=vout[S["lat0"]:S["lat0"] + n, t * 128 + 64 * h:t * 128 + 64 * h + 64].rearrange("(c j) v -> j c v", j=64),
                          in_=Vst[tp][hs, 0:nch, hs])
                if stop == 'd': continue
                for c in range(nch):
                    cp = c % 2
                    W_ = lambda nm: cw_[nm, cp]
                    k0 = ("w0", 0);
                    AH = ARx[tp][:, c, 0, :]; RP = ARx[tp][:, c, 1, :]; AR2 = ARx[tp][:, c, :, :].rearrange("p a j -> p (a j)")
                    BT_ = BTx[tp][:, c, :]; KT_ = KTx[tp][:, c, :]
                    I(P, "pe", "matmul", ps_w0[:, 0:256], lhsT=BT_, rhs=AR2, start=True, stop=True)
                    if stop == 'm1': continue
                    I(P, "pe", "matmul", ps_w1[:, 0:256], lhsT=KT_, rhs=AR2, start=True, stop=True)
                    if stop == 'm2': continue
                    I(P, "pe", "matmul", ps_inv[:, 0:128], lhsT=AH, rhs=BT_, start=True, stop=True)
                    if stop == 'e0': continue
                    I(P, "dve", "tensor_tensor", out=W_("U0A")[:], in0=ps_w0[:, 0:256], in1=M1b, op=ALU.mult)
                    I(P, "dve", "tensor_tensor", out=W_("AK")[:], in0=ps_w1[:, 0:256], in1=M1b, op=ALU.mult)
                    I(P, "dve", "tensor_tensor", out=W_("N0")[:], in0=ps_inv[:, 0:128], in1=SLb, op=ALU.mult)
                    U0 = W_("U0A")[:, 0:128]; ArbT = W_("U0A")[:, 128:256]; AakT = W_("AK")[:, 0:128]; ArkT = W_("AK")[:, 128:256]
                    if stop == 'e1': continue
                    slot = [0]
                    def nslot():
                        slot[0] = slot[0] % 3 + 1
                        return ps_inv[:, slot[0] * 128:(slot[0] + 1) * 128], f"pw1_{slot[0]}"
                    I(P, "dve", "tensor_tensor", out=W_("MT0")[:], in0=U0, in1=identb, op=ALU.add)
                    Ncur, Ucur, MTcur = W_("N0")[:], U0, W_("MT0")[:]
                    Nn = [W_("N1"), W_("N2")]; Un = [W_("U1"), W_("U2")]; MTn = [W_("MT1"), W_("MT0")]
                    for i in range(6):
                        if i >= 1:
                            o, k = nslot()
                            I(P, "pe", "matmul", o, lhsT=Ncur, rhs=MTcur, start=True, stop=True)
                            mt_new = MTn[(i - 1) % 2]
                            I(P, "dve", "tensor_tensor", out=mt_new[:], in0=o, in1=MTcur, op=ALU.add)
                            MTcur = mt_new[:]
                        if i <= 4:
                            o, k = nslot()
                            I(P, "pe", "matmul", o, lhsT=Ucur, rhs=Ncur, start=True, stop=True)
                            n_new = Nn[i % 2]
                            if i <= 3:
                                o2, k2 = nslot()
                                I(P, "pe", "matmul", o2, lhsT=Ncur, rhs=Ucur, start=True, stop=True)
                                u_new = Un[i % 2]
                                I(P, "act", "copy", out=u_new[:], in_=o2)
                                Ucur_next = u_new[:]
                            I(P, "act", "copy", out=n_new[:], in_=o)
                            Ncur = n_new[:]
                            if i <= 3:
                                Ucur = Ucur_next
                    if stop == 'e2': continue
                    Vc = Vb[tp][:, c, :]
                    I(P, "pe", "matmul", ps_w2[:, 0:128], lhsT=AH, rhs=Zb[t][:], start=True, stop=False)
                    I(P, "pe", "matmul", ps_w2[:, 0:128], lhsT=AakT, rhs=Vc, start=False, stop=True)
                    I(P, "act", "copy", out=W_("X")[:], in_=ps_w2[:, 0:128])
                    if stop == 'e3': continue
                    I(P, "pe", "matmul", ps_w2[:, 128:256], lhsT=MTcur, rhs=W_("X")[:], start=True, stop=True)
                    I(P, "act", "copy", out=W_("Pb")[:], in_=ps_w2[:, 128:256])
                    I(P, "pe", "matmul", ps_l2[:, 0:128], lhsT=RP, rhs=Zb[t][:], start=True, stop=False)
                    I(P, "pe", "matmul", ps_l2[:, 0:128], lhsT=ArbT, rhs=W_("Pb")[:], start=False, stop=False)
                    I(P, "pe", "matmul", ps_l2[:, 0:128], lhsT=ArkT, rhs=Vc, start=False, stop=True)
                    if stop == 'e4': continue
                    I(P, "pe", "matmul", ps_l[:, 0:128], lhsT=BDT[tp][:, c, :], rhs=W_("Pb")[:], start=True, stop=False)
                    I(P, "pe", "matmul", ps_l[:, 0:128], lhsT=KDT[tp][:, c, :], rhs=Vc, start=False, stop=True)
                    if not S["ctx"]:
                        I(P, "act", "copy", out=Yst[tp][:, c, :], in_=ps_l2[:, 0:128])
                    I(P, "dve", "scalar_tensor_tensor", out=Zf[t][:], in0=Zf[t][:], scalar=gC[tp][:, c:c + 1], in1=ps_l[:, 0:128], op0=ALU.mult, op1=ALU.add)
                    I(P, "act", "copy", out=Zb[t][:], in_=Zf[t][:])
                if not S["ctx"]:
                    for h in range(2):
                        hs = slice(64 * h, 64 * h + 64)
                        I(P, "sp", "dma_start", out=yout[S["lat0"]:S["lat0"] + n, t * 128 + 64 * h:t * 128 + 64 * h + 64].rearrange("(c j) v -> j c v", j=64),
                          in_=Yst[tp][hs, 0:nch, hs])
            if not S["ctx"]:
                I(P, "sp", "dma_start", out=sout[S["lat0"]:S["lat0"] + n, :].rearrange("(s p) h -> p s h", p=128), in_=sst[:, 0:n // 128, :])
        P.emit()
    return nc


A_SLAB2 = 3360
NTK = 2048
NTT = NTK // 128
ALPHA_ = 2.0 ** 0.25
GN_EPS_ = 64e-5


def prep2(inp, b, q, ph):
    tk = slice(NTK * q, NTK * q + NTK)
    w_in = inp["w_in"][0]
    def kcl(w):
        return np.ascontiguousarray(np.transpose(w.reshape(8, 128, -1), (1, 0, 2)))
    cols = np.concatenate([np.arange(A_SLAB2 + 1024, A_SLAB2 + 2048), np.arange(A_SLAB2 + 2048, A_SLAB2 + 4096)])
    w_ada = inp["w_ada"][0]
    m = dict(
        x2=np.ascontiguousarray(inp["x"][b][tk]),
        yf=np.ascontiguousarray(ph["yf"][b][tk]), yb=np.ascontiguousarray(ph["yb"][b][tk]),
        vv=np.ascontiguousarray(ph["v"][b][tk]), gg=np.ascontiguousarray(ph["g"][b][tk]),
        hf=np.ascontiguousarray(ph["hf"][b][tk]), hb=np.ascontiguousarray(ph["hb"][b][tk]),
        sfb=np.ascontiguousarray(np.concatenate([ph["sf"][b][tk], ph["sb"][b][tk]], 1)),
        cT2=np.ascontiguousarray(inp["c"][b].reshape(8, 128).T),
        wadaC=kcl(w_ada[:, :2048]), wadaR=kcl(w_ada[:, 2048:]),
        badaC=np.ascontiguousarray(inp["b_ada"][0][:2048].reshape(16, 128).T),
        badaR=np.ascontiguousarray(inp["b_ada"][0][2048:].reshape(1, 4096)),
        wslab=kcl(w_in[:, cols]), pa=kcl(inp["p_a"][0]), pb=kcl(inp["p_b"][0]), wo=kcl(inp["w_o"][0]),
        gains=np.ascontiguousarray(np.stack([inp["gn_g"][0], inp["gn_b"][0], inp["ln1_g"][0], inp["ln1_b"][0],
                                             inp["ln2_g"][0], inp["ln2_b"][0]])[None]),
        rw=kcl(np.concatenate([inp["router_g"][0], inp["router_e"][0]], 1)),
        rbias=np.concatenate([inp["router_g_b"][0], inp["router_e_b"][0]])[None].astype(np.float32),
        ew1=inp["e_w1"][0], ew3=inp["e_w3"][0], ew2=inp["e_w2"][0],
        ident=np.eye(128, dtype=np.float32),
    )
    return m


def build_p2(nc, stop=None, n_exp=32):
    P = Prog(nc)
    def din(name, shape):
        return nc.dram_tensor(name, shape, F32, kind="ExternalInput").ap()
    def dout(name, shape):
        return nc.dram_tensor(name, shape, F32, kind="ExternalOutput").ap()
    x2 = din("x2", [NTK, 1024]); yfd = din("yf", [NTK, 1024]); ybd = din("yb", [NTK, 1024]); vvd = din("vv", [NTK, 1024])
    ggd = din("gg", [NTK, 1024]); hfd = din("hf", [NTK, 1024]); hbd = din("hb", [NTK, 1024]); sfbd = din("sfb", [NTK, 32])
    cT2d = din("cT2", [128, 8]); wadaC = din("wadaC", [128, 8, 2048]); wadaR = din("wadaR", [128, 8, 4096])
    badaC = din("badaC", [128, 16]); badaR = din("badaR", [1, 4096])
    wslab = din("wslab", [128, 8, 3072]); pad = din("pa", [128, 8, 1024]); pbd = din("pb", [128, 8, 1024]); wod = din("wo", [128, 8, 1024])
    gains = din("gains", [1, 6, 1024]); rwd = din("rw", [128, 8, 36]); rbd = din("rbias", [1, 36])
    ew1 = din("ew1", [32, 1024, 512]); ew3 = din("ew3", [32, 1024, 512]); ew2 = din("ew2", [32, 512, 1024])
    identd = din("ident", [128, 128])
    outd = dout("out", [NTK, 1024]); gsc = dout("gsc", [NTK, 3072]); x1s = dout("x1s", [NTK, 1024])

    cnt = [0]
    def mk(stack):
        def sb(name, shape, dt=F32):
            cnt[0] += 1
            return stack.enter_context(nc.sbuf_tensor(f"s{cnt[0]}_{name}", shape, dt))
        def ps(name, shape, dt=F32):
            cnt[0] += 1
            return stack.enter_context(nc.psum_tensor(f"p{cnt[0]}_{name}", shape, dt))
        return sb, ps

    def layer_norm_stats(sb_x, stats, mv, rstd, eps):
        for hf_ in range(2):
            I(P, "dve", "bn_stats", out=stats[:, hf_, :], in_=sb_x[:, hf_ * 512:(hf_ + 1) * 512])
        I(P, "dve", "bn_aggr", out=mv[:], in_=stats[:])
        I(P, "act", "activation", out=rstd[:], in_=mv[:, 1:2], func=AF.Sqrt, bias=eps)
        I(P, "dve", "reciprocal", out=rstd[:], in_=rstd[:])

    common = contextlib.ExitStack()
    with common:
        sbc, psc = mk(common)
        idf = sbc("idf", [128, 128]); idb = sbc("idb", [128, 128], BF16)
        cTs = sbc("cTs", [128, 8]); cTa = sbc("cTa", [128, 8]); cTr = sbc("cTr", [128, 8, 128])
        modv = sbc("modv", [128, 16]); scp = sbc("scp", [128, 8])
        gt2 = sbc("gt2", [128, 1024])
        xmTb = sbc("xmTb", [128, 8, NTK], BF16)
        wts = sbc("wts", [128, NTT, 32])
        stats = sbc("stats", [128, 2, 6]); mv = sbc("mv", [128, 2]); rstd = sbc("rstd", [128, 1])

        def mod_row(dst, voff, wrp, bb, pst_):
            for j in range(4):
                c0 = voff * 1024 + j * 256
                I(P, "sp", "dma_start", out=wrp[:], in_=wadaR[:, :, c0:c0 + 256])
                I(P, "sp", "dma_start", out=bb[:], in_=badaR[:, c0:c0 + 256].partition_broadcast(128))
                for kc in range(8):
                    I(P, "pe", "matmul", pst_[:, 0:256], lhsT=cTr[:, kc, :], rhs=wrp[:, kc, :], start=(kc == 0), stop=(kc == 7))
                I(P, "dve", "tensor_tensor", out=dst[:, j * 256:(j + 1) * 256], in0=pst_[:, 0:256], in1=bb[:], op=ALU.add)

        s0 = contextlib.ExitStack()
        with s0:
            sb0, ps0 = mk(s0)
            wap = sb0("wap", [128, 8, 128]); bdc = sb0("bdc", [128, 16])
            wrp = sb0("wrp", [128, 8, 256]); bb = sb0("bb", [128, 256])
            ps_m = ps0("ps_m", [128, 512])
            I(P, "sp", "dma_start", out=idf[:], in_=identd)
            I(P, "dve", "tensor_copy", out=idb[:], in_=idf[:])
            I(P, "sp", "dma_start", out=cTs[:], in_=cT2d)
            I(P, "sp", "dma_start", out=bdc[:], in_=badaC)
            I(P, "act", "activation", out=cTa[:], in_=cTs[:], func=AF.Silu)
            I(P, "dve", "tensor_copy", out=cTr[:], in_=cTa[:].unsqueeze(2).to_broadcast([128, 8, 128]))
            for j in range(16):
                I(P, "sp", "dma_start", out=wap[:], in_=wadaC[:, :, j * 128:(j + 1) * 128])
                for kc in range(8):
                    I(P, "pe", "matmul", ps_m[:, j:j + 1], lhsT=wap[:, kc, :], rhs=cTa[:, kc:kc + 1], start=(kc == 0), stop=(kc == 7))
            I(P, "dve", "tensor_tensor", out=modv[:], in0=ps_m[:, 0:16], in1=bdc[:], op=ALU.add)
            I(P, "dve", "tensor_scalar", out=scp[:], in0=modv[:, 8:16], scalar1=1.0, scalar2=None, op0=ALU.add)
            mod_row(gt2, 3, wrp, bb, ps_m)
            P.flush()
        P.barrier()
        if stop == "setup":
            P.emit(); return nc

        sA1 = contextlib.ExitStack()
        with sA1:
            sb1, ps1 = mk(sA1)
            w2b = sb1("w2b", [128, 8, 3072], BF16)
            xt = [sb1(f"xt{i}", [128, 1024]) for i in range(2)]
            xn = sb1("xn", [128, 1024]); hT = sb1("hT", [128, 8, 128], BF16)
            gts = [sb1(f"gts{i}", [128, 3072]) for i in range(2)]
            ps_t = [ps1(f"ps_t{i}", [128, 512]) for i in range(2)]
            ps_s = [ps1(f"ps_s{i}", [128, 512]) for i in range(3)]
            for kc in range(8):
                I(P, "pool", "dma_start", out=w2b[:, kc, :], in_=wslab[:, kc, :])
            for tt in range(NTT):
                xb_ = xt[tt % 2]
                I(P, "sp", "dma_start", out=xb_[:], in_=x2[tt * 128:(tt + 1) * 128, :])
                layer_norm_stats(xb_, stats, mv, rstd, 1e-5)
                I(P, "dve", "tensor_scalar", out=xn[:], in0=xb_[:], scalar1=mv[:, 0:1], scalar2=rstd[:, 0:1], op0=ALU.subtract, op1=ALU.mult)
                for kc in range(8):
                    I(P, "pe", "transpose", ps_t[kc // 4][:, (kc % 4) * 128:(kc % 4 + 1) * 128], in_=xn[:, kc * 128:(kc + 1) * 128], identity=idf[:])
                for kc in range(8):
                    pso = ps_t[kc // 4][:, (kc % 4) * 128:(kc % 4 + 1) * 128]
                    if kc % 2 == 0:
                        I(P, "dve", "tensor_scalar", out=hT[:, kc, :], in0=pso, scalar1=scp[:, kc:kc + 1], scalar2=modv[:, kc:kc + 1], op0=ALU.mult, op1=ALU.add)
                    else:
                        I(P, "act", "activation", out=hT[:, kc, :], in_=pso, func=AF.Identity, scale=scp[:, kc:kc + 1], bias=modv[:, kc:kc + 1])
                g_ = gts[tt % 2]
                for blk in range(6):
                    pt = ps_s[blk % 3]
                    for kc in range(8):
                        I(P, "pe", "matmul", pt[:, :], lhsT=hT[:, kc, :], rhs=w2b[:, kc, blk * 512:(blk + 1) * 512], start=(kc == 0), stop=(kc == 7))
                    I(P, "act", "activation", out=g_[:, blk * 512:(blk + 1) * 512], in_=pt[:, :], func=(AF.Gelu if blk < 2 else AF.Sigmoid))
                I(P, "sp", "dma_start", out=gsc[tt * 128:(tt + 1) * 128, :], in_=g_[:])
            P.flush()
        P.barrier()
        if stop == "a1":
            P.emit(); return nc

        sA2 = contextlib.ExitStack()
        with sA2:
            sb2, ps2 = mk(sA2)
            pab = sb2("pab", [128, 8, 1024], BF16); pbb = sb2("pbb", [128, 8, 1024], BF16); wob = sb2("wob", [128, 8, 1024], BF16)
            gt1 = sb2("gt1", [128, 1024]); sh2 = sb2("sh2", [128, 1024]); sc2p = sb2("sc2p", [128, 1024])
            wrp = sb2("wrp", [128, 8, 256]); bb = sb2("bb", [128, 256])
            gng = sb2("gng", [128, 1024]); gnb = sb2("gnb", [128, 1024]); l1g = sb2("l1g", [128, 1024]); l1b = sb2("l1b", [128, 1024])
            rwf = sb2("rwf", [128, 8, 36]); rbs = sb2("rbs", [128, 36])
            xt = sb2("xt", [128, 1024]); xn = sb2("xn", [128, 1024])
            yft = sb2("yft", [128, 1024]); ybt = sb2("ybt", [128, 1024]); vt = sb2("vt", [128, 1024]); gt_ = sb2("gt_", [128, 1024])
            hft = sb2("hft", [128, 1024]); hbt = sb2("hbt", [128, 1024]); sft = sb2("sft", [128, 32]); gts = sb2("gts", [128, 3072])
            ta = sb2("ta", [128, 1024]); tb_ = sb2("tb", [128, 1024])
            yab = sb2("yab", [128, 1024], BF16); ybb = sb2("ybb", [128, 1024], BF16); mb = sb2("mb", [128, 1024], BF16)
            yaT = sb2("yaT", [128, 8, 128], BF16); ybT = sb2("ybT", [128, 8, 128], BF16); mT = sb2("mT", [128, 8, 128], BF16)
            xmTf = sb2("xmTf", [128, 8, 128])
            st16 = sb2("st16", [128, 16, 6]); mv16 = sb2("mv16", [128, 16, 2]); rs16 = sb2("rs16", [128, 16]); ss16 = sb2("ss16", [128, 16])
            R = {n: sb2("r_" + n, [128, 32]) for n in ["lg", "ohg", "eg", "pen", "m32", "oh1", "m32b", "oh2", "W"]}
            r1 = {n: sb2("q_" + n, [128, 1]) for n in ["gmax", "ngmax", "sumg", "pg", "t1", "t2", "d", "ed", "w1", "w2"]}
            ps_t = [ps2(f"ps_t{i}", [128, 512]) for i in range(2)]
            pst = ps2("pst", [128, 1024], BF16)
            ps_a = [ps2(f"ps_a{i}", [128, 512]) for i in range(2)]
            ps_b = [ps2(f"ps_b{i}", [128, 512]) for i in range(2)]
            ps_r = ps2("ps_r", [128, 512])
            for (dst, src) in ((pab, pad), (pbb, pbd), (wob, wod)):
                for kc in range(0, 8, 4):
                    I(P, "pool", "dma_start", out=dst[:, kc:kc + 4, :], in_=src[:, kc:kc + 4, :])
            for i_, dst in enumerate((gng, gnb, l1g, l1b)):
                I(P, "sp", "dma_start", out=dst[:], in_=gains[:, i_, :].partition_broadcast(128))
            I(P, "sp", "dma_start", out=rwf[:], in_=rwd)
            I(P, "sp", "dma_start", out=rbs[:], in_=rbd.partition_broadcast(128))
            mod_row(gt1, 0, wrp, bb, ps_r)
            mod_row(sh2, 1, wrp, bb, ps_r)
            mod_row(sc2p, 2, wrp, bb, ps_r)
            I(P, "dve", "tensor_scalar", out=sc2p[:], in0=sc2p[:], scalar1=1.0, scalar2=None, op0=ALU.add)
            for tt in range(NTT):
                rows = slice(tt * 128, (tt + 1) * 128)
                for (dst, src) in ((xt, x2), (yft, yfd), (ybt, ybd), (vt, vvd), (gt_, ggd), (hft, hfd), (hbt, hbd), (sft, sfbd), (gts, gsc)):
                    I(P, "sp", "dma_start", out=dst[:], in_=src[rows, :])
                layer_norm_stats(xt, stats, mv, rstd, 1e-5)
                I(P, "dve", "tensor_scalar", out=xn[:], in0=xt[:], scalar1=mv[:, 0:1], scalar2=rstd[:, 0:1], op0=ALU.subtract, op1=ALU.mult)
                if stop == 'L1': continue
                I(P, "pool", "tensor_tensor", out=ta[:], in0=yft[:], in1=ybt[:], op=ALU.add)
                for h in range(16):
                    I(P, "dve", "bn_stats", out=st16[:, h, :], in_=ta[:, h * 64:(h + 1) * 64])
                for h in range(16):
                    I(P, "dve", "bn_aggr", out=mv16[:, h, :], in_=st16[:, h, :])
                I(P, "act", "activation", out=rs16[:], in_=mv16[:, :, 1], func=AF.Sqrt, bias=GN_EPS_)
                I(P, "dve", "reciprocal", out=rs16[:], in_=rs16[:])
                ta3 = ta[:].rearrange("p (h c) -> p h c", c=64)
                I(P, "dve", "tensor_tensor", out=ta3, in0=ta3, in1=mv16[:, :, 0:1].to_broadcast([128, 16, 64]), op=ALU.subtract)
                I(P, "dve", "tensor_tensor", out=ta3, in0=ta3, in1=rs16[:].unsqueeze(2).to_broadcast([128, 16, 64]), op=ALU.mult)
                I(P, "pool", "tensor_tensor", out=ta[:], in0=ta[:], in1=gng[:], op=ALU.mult)
                I(P, "pool", "tensor_tensor", out=ta[:], in0=ta[:], in1=gnb[:], op=ALU.add)
                I(P, "dve", "tensor_tensor", out=ss16[:], in0=sft[:, 0:16], in1=sft[:, 16:32], op=ALU.add)
                I(P, "dve", "tensor_tensor", out=tb_[:].rearrange("p (h c) -> p h c", c=64), in0=vt[:].rearrange("p (h c) -> p h c", c=64),
                  in1=ss16[:].unsqueeze(2).to_broadcast([128, 16, 64]), op=ALU.mult)
                I(P, "pool", "tensor_tensor", out=ta[:], in0=ta[:], in1=tb_[:], op=ALU.add)
                I(P, "dve", "tensor_tensor", out=yab[:], in0=ta[:], in1=gt_[:], op=ALU.mult)
                if stop == 'L2': continue
                I(P, "pool", "tensor_tensor", out=tb_[:], in0=hft[:], in1=hbt[:], op=ALU.add)
                I(P, "pool", "tensor_tensor", out=ybb[:], in0=tb_[:], in1=gts[:, 0:1024], op=ALU.mult)
                if stop == 'L3': continue
                for (src, dst) in ((yab, yaT), (ybb, ybT)):
                    for kc in range(8):
                        I(P, "pe", "transpose", pst[:, kc * 128:(kc + 1) * 128], in_=src[:, kc * 128:(kc + 1) * 128], identity=idb[:])
                    I(P, "act", "copy", out=dst[:], in_=pst[:, :].rearrange("p (k j) -> p k j", j=128))
                for hf_ in range(2):
                    for kc in range(8):
                        I(P, "pe", "matmul", ps_a[hf_][:, :], lhsT=yaT[:, kc, :], rhs=pab[:, kc, hf_ * 512:(hf_ + 1) * 512], start=(kc == 0), stop=(kc == 7))
                    for kc in range(8):
                        I(P, "pe", "matmul", ps_b[hf_][:, :], lhsT=ybT[:, kc, :], rhs=pbb[:, kc, hf_ * 512:(hf_ + 1) * 512], start=(kc == 0), stop=(kc == 7))
                    cs = slice(hf_ * 512, (hf_ + 1) * 512)
                    I(P, "dve", "tensor_tensor", out=ta[:, cs], in0=ps_a[hf_][:, :], in1=gts[:, 1024 + hf_ * 512:1024 + (hf_ + 1) * 512], op=ALU.mult)
                    I(P, "dve", "tensor_tensor", out=tb_[:, cs], in0=ps_b[hf_][:, :], in1=gts[:, 2048 + hf_ * 512:2048 + (hf_ + 1) * 512], op=ALU.mult)
                I(P, "pool", "tensor_tensor", out=mb[:], in0=ta[:], in1=tb_[:], op=ALU.add)
                for kc in range(8):
                    I(P, "pe", "transpose", pst[:, kc * 128:(kc + 1) * 128], in_=mb[:, kc * 128:(kc + 1) * 128], identity=idb[:])
                I(P, "act", "copy", out=mT[:], in_=pst[:, :].rearrange("p (k j) -> p k j", j=128))
                for hf_ in range(2):
                    for kc in range(8):
                        I(P, "pe", "matmul", ps_a[hf_][:, :], lhsT=mT[:, kc, :], rhs=wob[:, kc, hf_ * 512:(hf_ + 1) * 512], start=(kc == 0), stop=(kc == 7))
                    cs = slice(hf_ * 512, (hf_ + 1) * 512)
                    I(P, "dve", "tensor_tensor", out=ta[:, cs], in0=ps_a[hf_][:, :], in1=gt1[:, cs], op=ALU.mult)
                if stop == 'L4': continue
                I(P, "dve", "scalar_tensor_tensor", out=ta[:], in0=xn[:], scalar=ALPHA_, in1=ta[:], op0=ALU.mult, op1=ALU.add)
                layer_norm_stats(ta, stats, mv, rstd, 1e-5)
                I(P, "dve", "tensor_scalar", out=ta[:], in0=ta[:], scalar1=mv[:, 0:1], scalar2=rstd[:, 0:1], op0=ALU.subtract, op1=ALU.mult)
                I(P, "pool", "tensor_tensor", out=ta[:], in0=ta[:], in1=l1g[:], op=ALU.mult)
                I(P, "pool", "tensor_tensor", out=ta[:], in0=ta[:], in1=l1b[:], op=ALU.add)
                I(P, "sp", "dma_start", out=x1s[rows, :], in_=ta[:])
                if stop == 'L5': continue
                I(P, "dve", "tensor_tensor", out=tb_[:], in0=ta[:], in1=sc2p[:], op=ALU.mult)
                I(P, "dve", "tensor_tensor", out=tb_[:], in0=tb_[:], in1=sh2[:], op=ALU.add)
                for kc in range(8):
                    I(P, "pe", "transpose", ps_t[kc // 4][:, (kc % 4) * 128:(kc % 4 + 1) * 128], in_=tb_[:, kc * 128:(kc + 1) * 128], identity=idf[:])
                for j in range(2):
                    pv = ps_t[j][:, :].rearrange("p (k j) -> p k j", j=128)
                    I(P, "act", "copy", out=xmTf[:, 4 * j:4 * j + 4, :], in_=pv)
                    I(P, "pool", "tensor_copy", out=xmTb[:, 4 * j:4 * j + 4, tt * 128:(tt + 1) * 128], in_=xmTf[:, 4 * j:4 * j + 4, :])
                if stop == 'L6': continue
                for kc in range(8):
                    I(P, "pe", "matmul", ps_r[:, 0:36], lhsT=xmTf[:, kc, :], rhs=rwf[:, kc, :], start=(kc == 0), stop=(kc == 7))
                lg = R["lg"]
                I(P, "dve", "tensor_tensor", out=lg[:, 0:36] if False else R["lg"][:, 0:32], in0=ps_r[:, 4:36], in1=rbs[:, 4:36], op=ALU.add)
                I(P, "dve", "tensor_tensor", out=R["eg"][:, 0:4], in0=ps_r[:, 0:4], in1=rbs[:, 0:4], op=ALU.add)
                gl = R["eg"][:, 0:4]
                if stop == 'L7': continue
                I(P, "dve", "tensor_reduce", out=r1["gmax"][:], in_=gl, axis=AX.X, op=ALU.max)
                I(P, "dve", "tensor_scalar", out=R["ohg"][:, 0:4], in0=gl, scalar1=r1["gmax"][:, 0:1], scalar2=None, op0=ALU.is_equal)
                I(P, "dve", "tensor_scalar", out=r1["ngmax"][:], in0=r1["gmax"][:], scalar1=-1.0, scalar2=None, op0=ALU.mult)
                I(P, "act", "activation", out=R["eg"][:, 4:8], in_=gl, func=AF.Exp, bias=r1["ngmax"][:, 0:1])
                I(P, "dve", "tensor_reduce", out=r1["sumg"][:], in_=R["eg"][:, 4:8], axis=AX.X, op=ALU.add)
                I(P, "dve", "reciprocal", out=r1["pg"][:], in_=r1["sumg"][:])
                I(P, "dve", "tensor_scalar", out=R["pen"][:, 0:4], in0=R["ohg"][:, 0:4], scalar1=-1.0, scalar2=1e30, op0=ALU.add, op1=ALU.mult)
                I(P, "dve", "tensor_tensor", out=R["m32"][:].rearrange("p (g e) -> p g e", e=8), in0=R["lg"][:].rearrange("p (g e) -> p g e", e=8),
                  in1=R["pen"][:, 0:4].unsqueeze(2).to_broadcast([128, 4, 8]), op=ALU.add)
                I(P, "dve", "tensor_reduce", out=r1["t1"][:], in_=R["m32"][:], axis=AX.X, op=ALU.max)
                I(P, "dve", "tensor_scalar", out=R["oh1"][:], in0=R["m32"][:], scalar1=r1["t1"][:, 0:1], scalar2=None, op0=ALU.is_equal)
                I(P, "dve", "scalar_tensor_tensor", out=R["m32b"][:], in0=R["oh1"][:], scalar=-1e30, in1=R["m32"][:], op0=ALU.mult, op1=ALU.add)
                I(P, "dve", "tensor_reduce", out=r1["t2"][:], in_=R["m32b"][:], axis=AX.X, op=ALU.max)
                I(P, "dve", "tensor_scalar", out=R["oh2"][:], in0=R["m32b"][:], scalar1=r1["t2"][:, 0:1], scalar2=None, op0=ALU.is_equal)
                I(P, "dve", "tensor_tensor", out=r1["d"][:], in0=r1["t2"][:], in1=r1["t1"][:], op=ALU.subtract)
                I(P, "act", "activation", out=r1["ed"][:], in_=r1["d"][:], func=AF.Exp)
                I(P, "dve", "tensor_scalar", out=r1["w1"][:], in0=r1["ed"][:], scalar1=1.0, scalar2=None, op0=ALU.add)
                I(P, "dve", "reciprocal", out=r1["w1"][:], in_=r1["w1"][:])
                I(P, "dve", "tensor_tensor", out=r1["w2"][:], in0=r1["ed"][:], in1=r1["w1"][:], op=ALU.mult)
                I(P, "dve", "tensor_tensor", out=r1["w1"][:], in0=r1["w1"][:], in1=r1["pg"][:], op=ALU.mult)
                I(P, "dve", "tensor_tensor", out=r1["w2"][:], in0=r1["w2"][:], in1=r1["pg"][:], op=ALU.mult)
                I(P, "dve", "tensor_scalar", out=R["W"][:], in0=R["oh1"][:], scalar1=r1["w1"][:, 0:1], scalar2=None, op0=ALU.mult)
                I(P, "dve", "scalar_tensor_tensor", out=wts[:, tt, :], in0=R["oh2"][:], scalar=r1["w2"][:, 0:1], in1=R["W"][:], op0=ALU.mult, op1=ALU.add)
            P.flush()
        P.barrier()
        if stop == "a2" or (stop or "").startswith("L"):
            P.emit(); return nc

        sB = contextlib.ExitStack()
        with sB:
            sb3, ps3 = mk(sB)
            acc = [sb3(f"acc{i}", [128, 1024]) for i in range(NTT)]
            w1b = sb3("w1b", [128, 8, 512], BF16); w3b = sb3("w3b", [128, 8, 512], BF16); w2bb = sb3("w2bb", [128, 4, 1024], BF16)
            stg = [sb3(f"stg{i}", [128, 2048]) for i in range(4)]
            hid = [sb3(f"hid{i}", [128, 4, 512], BF16) for i in range(4)]
            sil = [sb3(f"sil{i}", [128, 512]) for i in range(2)]
            l2g = sb3("l2g", [128, 1024]); l2b = sb3("l2b", [128, 1024])
            x1t = sb3("x1t", [128, 1024]); pre = sb3("pre", [128, 1024])
            ps_h1 = [ps3(f"ps_h1{i}", [128, 512]) for i in range(2)]
            ps_h3 = [ps3(f"ps_h3{i}", [128, 512]) for i in range(2)]
            ps_o = [ps3(f"ps_o{i}", [128, 512]) for i in range(4)]
            I(P, "sp", "dma_start", out=l2g[:], in_=gains[:, 4, :].partition_broadcast(128))
            I(P, "sp", "dma_start", out=l2b[:], in_=gains[:, 5, :].partition_broadcast(128))
            si = [0]
            def load13(e):
                for (wd, dstb) in ((ew1, w1b), (ew3, w3b)):
                    for pc in range(2):
                        s_ = stg[si[0] % 4]; si[0] += 1
                        I(P, "sp", "dma_start", out=s_[:].rearrange("p (k f) -> p k f", f=512),
                          in_=wd[e, pc * 512:(pc + 1) * 512, :].rearrange("(k p) f -> p k f", p=128))
                        I(P, "act", "copy", out=dstb[:, 4 * pc:4 * pc + 4, :], in_=s_[:].rearrange("p (k f) -> p k f", f=512))
            def load2(e):
                for pc in range(2):
                    s_ = stg[si[0] % 4]; si[0] += 1
                    I(P, "sp", "dma_start", out=s_[:].rearrange("p (k f) -> p k f", f=1024),
                      in_=ew2[e, pc * 256:(pc + 1) * 256, :].rearrange("(k p) f -> p k f", p=128))
                    I(P, "pool", "tensor_copy", out=w2bb[:, 2 * pc:2 * pc + 2, :], in_=s_[:].rearrange("p (k f) -> p k f", f=1024))
            load13(0); load2(0)
            hi = [0]; oi = [0]
            for e in range(n_exp):
                for tb in range(4):
                    for fc in range(4):
                        p1_ = ps_h1[hi[0] % 2]; p3_ = ps_h3[hi[0] % 2]; sl_ = sil[hi[0] % 2]; hi[0] += 1
                        for kc in range(8):
                            I(P, "pe", "matmul", p1_[:, :], lhsT=w1b[:, kc, fc * 128:(fc + 1) * 128], rhs=xmTb[:, kc, tb * 512:(tb + 1) * 512], start=(kc == 0), stop=(kc == 7))
                        for kc in range(8):
                            I(P, "pe", "matmul", p3_[:, :], lhsT=w3b[:, kc, fc * 128:(fc + 1) * 128], rhs=xmTb[:, kc, tb * 512:(tb + 1) * 512], start=(kc == 0), stop=(kc == 7))
                        I(P, "act", "activation", out=sl_[:], in_=p1_[:, :], func=AF.Silu)
                        I(P, "dve", "tensor_tensor", out=hid[tb][:, fc, :], in0=sl_[:], in1=p3_[:, :], op=ALU.mult)
                if e + 1 < n_exp:
                    load13(e + 1)
                for tt in range(NTT):
                    tb, ti = tt // 4, tt % 4
                    for hf_ in range(2):
                        po = ps_o[oi[0] % 4]; oi[0] += 1
                        for fc in range(4):
                            I(P, "pe", "matmul", po[:, :], lhsT=hid[tb][:, fc, ti * 128:(ti + 1) * 128], rhs=w2bb[:, fc, hf_ * 512:(hf_ + 1) * 512], start=(fc == 0), stop=(fc == 3))
                        cs = slice(hf_ * 512, (hf_ + 1) * 512)
                        if e == 0:
                            I(P, "dve", "tensor_scalar", out=acc[tt][:, cs], in0=po[:, :], scalar1=wts[:, tt, e:e + 1], scalar2=None, op0=ALU.mult)
                        else:
                            I(P, "dve", "scalar_tensor_tensor", out=acc[tt][:, cs], in0=po[:, :], scalar=wts[:, tt, e:e + 1], in1=acc[tt][:, cs], op0=ALU.mult, op1=ALU.add)
                if e + 1 < n_exp:
                    load2(e + 1)
            for tt in range(NTT):
                rows = slice(tt * 128, (tt + 1) * 128)
                I(P, "sp", "dma_start", out=x1t[:], in_=x1s[rows, :])
                I(P, "dve", "tensor_tensor", out=pre[:], in0=acc[tt][:], in1=gt2[:], op=ALU.mult)
                I(P, "dve", "scalar_tensor_tensor", out=pre[:], in0=x1t[:], scalar=ALPHA_, in1=pre[:], op0=ALU.mult, op1=ALU.add)
                layer_norm_stats(pre, stats, mv, rstd, 1e-5)
                I(P, "dve", "tensor_scalar", out=pre[:], in0=pre[:], scalar1=mv[:, 0:1], scalar2=rstd[:, 0:1], op0=ALU.subtract, op1=ALU.mult)
                I(P, "pool", "tensor_tensor", out=pre[:], in0=pre[:], in1=l2g[:], op=ALU.mult)
                I(P, "pool", "tensor_tensor", out=pre[:], in0=pre[:], in1=l2b[:], op=ALU.add)
                I(P, "sp", "dma_start", out=outd[rows, :], in_=pre[:])
            P.flush()
        P.emit()
    return nc


def kernel(**inputs):
    inp = {k: np.asarray(v) for k, v in inputs.items()}
    maps = []
    for core in range(8):
        b, e, hh = core // 4, (core // 2) % 2, core % 2
        maps.append(prep1(inp, b, e, hh))
    nc1 = bass.Bass("TRN2", target_bir_lowering=False)
    build_p1(nc1, n_lat_sb=16)
    r1 = run_bass_kernel_spmd(nc1, maps, core_ids=list(range(8))).results
    del maps
    ph = {k: [None, None] for k in ["yf", "yb", "v", "g", "hf", "hb", "sf", "sb"]}
    for b in range(2):
        f = [r1[b * 4 + hh] for hh in range(2)]
        k = [r1[b * 4 + 2 + hh] for hh in range(2)]
        ph["yf"][b] = np.concatenate([c["yout"] for c in f], 1)
        ph["yb"][b] = np.concatenate([c["yout"][::-1] for c in k], 1)
        ph["v"][b] = np.concatenate([c["vout"] for c in f], 1)
        ph["g"][b] = np.concatenate([c["gout"] for c in f], 1)
        ph["sf"][b] = np.concatenate([c["sout"] for c in f], 1)
        ph["sb"][b] = np.concatenate([c["sout"][::-1] for c in k], 1)
        ph["hf"][b] = np.concatenate([c["hout"].T for c in f], 1)
        ph["hb"][b] = np.concatenate([c["hout"].T[::-1] for c in k], 1)
    maps2 = [prep2(inp, core // 4, core % 4, ph) for core in range(8)]
    nc2 = bass.Bass("TRN2", target_bir_lowering=False)
    build_p2(nc2)
    r2 = run_bass_kernel_spmd(nc2, maps2, core_ids=list(range(8))).results
    out = np.empty((2, 8192, 1024), np.float32)
    for core in range(8):
        out[core // 4, (core % 4) * NTK:(core % 4 + 1) * NTK] = r2[core]["out"]
    return out
```
